# Optimizing a Trainium2 kernel written in Bass

```python
import jax, jax.numpy as jnp
from jax import lax
import numpy as np

D_MODEL = 4096
BATCH = 2
SEQ = 8192
DEPTH = 1

CHUNK = 64
QUERY_BLOCK = 128
LN_EPS = 1e-5
FOX_HEAD_DIM = 128
FOX_WIDTH = D_MODEL // 2
FOX_HEADS = FOX_WIDTH // FOX_HEAD_DIM
ML_HEADS = 4
ML_QK_DIM = 256
ML_V_DIM = 512
ML_QK_WIDTH = ML_HEADS * ML_QK_DIM
ML_V_WIDTH = ML_HEADS * ML_V_DIM
CONV_WIDTH = 4
N_GROUPS = 8
EXPERTS_PER_GROUP = 8
N_EXPERTS = N_GROUPS * EXPERTS_PER_GROUP
TOP_K = 2
D_EXPERT = 512
EXPERT_BLOCK = 128
DEEPNORM_ALPHA = (2 * DEPTH) ** 0.25
DEEPNORM_BETA = (8 * DEPTH) ** -0.25
IN_SPLIT_WIDTHS = (FOX_WIDTH, FOX_WIDTH, FOX_WIDTH, FOX_HEADS,
                   ML_QK_WIDTH, ML_QK_WIDTH, ML_V_WIDTH, ML_HEADS, ML_HEADS, ML_V_WIDTH,
                   D_MODEL, D_MODEL)
IN_WIDTH = sum(IN_SPLIT_WIDTHS)
IN_SPLIT_POINTS = tuple(int(p) for p in np.cumsum(IN_SPLIT_WIDTHS)[:-1])

kernel_name = "fox_mlstm_hier_moe_deepnorm"


def layer_norm(x, g, b):
    xf = x.astype(jnp.float32)
    mu = jnp.mean(xf, axis=-1, keepdims=True)
    var = jnp.mean(jnp.square(xf - mu), axis=-1, keepdims=True)
    return ((xf - mu) * lax.rsqrt(var + LN_EPS) * g + b).astype(x.dtype)


def causal_depthwise_conv(x, w, b):
    k_w = w.shape[0]
    s = x.shape[1]
    xp = jnp.pad(x, ((0, 0), (k_w - 1, 0), (0, 0)))
    return sum(w[j] * xp[:, j:j + s] for j in range(k_w)) + b


def forgetting_attention(q, k, v, log_f):
    b_, s, h, dh = q.shape
    c = jnp.cumsum(log_f, axis=1).transpose(0, 2, 1)
    n_blk = s // QUERY_BLOCK
    qb = q.reshape(b_, n_blk, QUERY_BLOCK, h, dh).transpose(1, 0, 2, 3, 4)
    cb = c.reshape(b_, h, n_blk, QUERY_BLOCK).transpose(2, 0, 1, 3)
    starts = jnp.arange(n_blk, dtype=jnp.int32) * QUERY_BLOCK
    k_pos = jnp.arange(s, dtype=jnp.int32)
    scale = FOX_HEAD_DIM ** -0.5

    def block(args):
        q_blk, c_blk, start = args
        logits = jnp.einsum('bqhd,bkhd->bhqk', q_blk, k,
                            preferred_element_type=jnp.float32) * scale
        logits = logits + (c_blk[..., :, None] - c[..., None, :])
        q_pos = start + jnp.arange(QUERY_BLOCK, dtype=jnp.int32)
        logits = jnp.where(k_pos[None, :] <= q_pos[:, None], logits, -jnp.inf)
        p = jax.nn.softmax(logits, axis=-1)
        return jnp.einsum('bhqk,bkhd->bqhd', p.astype(v.dtype), v)

    out = lax.map(block, (qb, cb, starts))
    return out.transpose(1, 0, 2, 3, 4).reshape(b_, s, h * dh)


def mlstm_chunkwise(q, k, v, i_pre, log_f):
    b_, s, h, dk = q.shape
    dv = v.shape[-1]
    nc = s // CHUNK

    def to_chunks(t):
        t = t.reshape(b_, nc, CHUNK, h, *t.shape[3:])
        return jnp.moveaxis(jnp.moveaxis(t, 1, 0), 3, 2)

    f32 = jnp.float32
    xs = (to_chunks(q.astype(f32)), to_chunks(k.astype(f32)), to_chunks(v.astype(f32)),
          to_chunks(i_pre), to_chunks(log_f))
    tri = jnp.tril(jnp.ones((CHUNK, CHUNK), dtype=bool))

    def step(carry, chunk):
        c_mem, n_mem, m_prev = carry
        qc, kc, vc, ic, fc = chunk
        bcum = jnp.cumsum(fc, axis=-1)
        d = bcum[..., :, None] - bcum[..., None, :] + ic[..., None, :]
        d = jnp.where(tri, d, -jnp.inf)
        inter = bcum + m_prev[..., None]
        m = jnp.maximum(inter, jnp.max(d, axis=-1))
        w_intra = jnp.exp(d - m[..., None])
        w_inter = jnp.exp(inter - m)
        sc = jnp.einsum('bhtd,bhsd->bhts', qc, kc) * w_intra
        num = (jnp.einsum('bhts,bhsv->bhtv', sc, vc)
               + w_inter[..., None] * jnp.einsum('bhtd,bhdv->bhtv', qc, c_mem))
        den = jnp.sum(sc, axis=-1) + w_inter * jnp.einsum('bhtd,bhd->bht', qc, n_mem)
        h_out = num / jnp.maximum(jnp.abs(den), jnp.exp(-m))[..., None]
        b_last = bcum[..., -1]
        g = b_last[..., None] - bcum + ic
        m_new = jnp.maximum(b_last + m_prev, jnp.max(g, axis=-1))
        w_k = jnp.exp(g - m_new[..., None])
        decay = jnp.exp(b_last + m_prev - m_new)
        c_new = decay[..., None, None] * c_mem + jnp.einsum('bhs,bhsd,bhsv->bhdv', w_k, kc, vc)
        n_new = decay[..., None] * n_mem + jnp.einsum('bhs,bhsd->bhd', w_k, kc)
        return (c_new, n_new, m_new), h_out

    init = (jnp.zeros((b_, h, dk, dv), f32), jnp.zeros((b_, h, dk), f32), jnp.zeros((b_, h), f32))
    _, hs = lax.scan(step, init, xs)
    return hs.transpose(1, 0, 3, 2, 4).reshape(b_, s, h, dv)


def hybrid_mixer(h, w_in, b_fox_f, b_ml_i, b_ml_f, conv_w, conv_b, ml_norm_g,
                 w_proj_fox, w_proj_ml, w_out):
    b_, s, _ = h.shape
    f32 = jnp.float32
    proj = jnp.einsum('bsd,de->bse', h, w_in)
    fq, fk, fv, ff, mq, mk, mv, mi, mf, mo, ga, gb = jnp.split(proj, IN_SPLIT_POINTS, axis=-1)

    log_f_fox = jax.nn.log_sigmoid((ff + b_fox_f).astype(f32))
    fox_shape = (b_, s, FOX_HEADS, FOX_HEAD_DIM)
    y_fox = forgetting_attention(fq.reshape(fox_shape), fk.reshape(fox_shape),
                                 fv.reshape(fox_shape), log_f_fox)

    qk = jax.nn.silu(causal_depthwise_conv(jnp.concatenate([mq, mk], axis=-1), conv_w, conv_b))
    mq, mk = jnp.split(qk, 2, axis=-1)
    qk_shape = (b_, s, ML_HEADS, ML_QK_DIM)
    h_ml = mlstm_chunkwise(mq.reshape(qk_shape), mk.reshape(qk_shape) * (ML_QK_DIM ** -0.5),
                           mv.reshape(b_, s, ML_HEADS, ML_V_DIM),
                           (mi + b_ml_i).astype(f32),
                           jax.nn.log_sigmoid((mf + b_ml_f).astype(f32)))
    mu = jnp.mean(h_ml, axis=-1, keepdims=True)
    var = jnp.mean(jnp.square(h_ml - mu), axis=-1, keepdims=True)
    h_ml = ((h_ml - mu) * lax.rsqrt(var + LN_EPS)).reshape(b_, s, ML_V_WIDTH)
    y_ml = (h_ml * ml_norm_g * jax.nn.sigmoid(mo.astype(f32))).astype(h.dtype)

    merged = (jax.nn.sigmoid(ga) * jnp.einsum('bsc,cd->bsd', y_fox, w_proj_fox)
              + jax.nn.sigmoid(gb) * jnp.einsum('bsc,cd->bsd', y_ml, w_proj_ml))
    return jnp.einsum('bsd,de->bse', merged, w_out)


def hierarchical_moe(h, w_group, b_group, w_router, b_router, w_gate, w_up, w_down):
    b_, s, d = h.shape
    xt = h.reshape(-1, d)
    n_tok = xt.shape[0]
    f32 = jnp.float32
    group_logits = (xt @ w_group + b_group).astype(f32)
    p_group = jax.nn.softmax(group_logits, axis=-1)
    g_sel = jnp.argmax(group_logits, axis=-1).astype(jnp.int32)
    p_g_sel = jnp.take_along_axis(p_group, g_sel[:, None], axis=-1)
    e_logits = (xt @ w_router + b_router).astype(f32).reshape(n_tok, N_GROUPS, EXPERTS_PER_GROUP)
    e_logits = jnp.take_along_axis(e_logits, g_sel[:, None, None], axis=1)[:, 0]
    top_p, top_i = lax.top_k(jax.nn.softmax(e_logits, axis=-1), TOP_K)
    gate = p_g_sel * top_p / jnp.sum(top_p, axis=-1, keepdims=True)
    expert_id = g_sel[:, None] * EXPERTS_PER_GROUP + top_i.astype(jnp.int32)

    n_assign = n_tok * TOP_K
    flat_e = expert_id.reshape(-1)
    flat_w = gate.reshape(-1)
    flat_tok = jnp.repeat(jnp.arange(n_tok, dtype=jnp.int32), TOP_K)
    order = jnp.argsort(flat_e)
    se, stok, sw = flat_e[order], flat_tok[order], flat_w[order]
    counts = jnp.bincount(flat_e, length=N_EXPERTS).astype(jnp.int32)
    padded = (counts + EXPERT_BLOCK - 1) // EXPERT_BLOCK * EXPERT_BLOCK
    starts = jnp.cumsum(counts) - counts
    pad_ends = jnp.cumsum(padded)
    pad_starts = pad_ends - padded
    dest = pad_starts[se] + (jnp.arange(n_assign, dtype=jnp.int32) - starts[se])
    n_blocks = (n_assign + N_EXPERTS * (EXPERT_BLOCK - 1) + EXPERT_BLOCK - 1) // EXPERT_BLOCK
    n_slots = n_blocks * EXPERT_BLOCK
    slot_tok = jnp.zeros((n_slots,), jnp.int32).at[dest].set(stok)
    slot_w = jnp.zeros((n_slots,), f32).at[dest].set(sw)
    block_e = jnp.minimum(
        jnp.searchsorted(pad_ends, jnp.arange(n_blocks, dtype=jnp.int32) * EXPERT_BLOCK, side='right'),
        N_EXPERTS - 1)

    def block(y, args):
        tok, wt, e = args
        xb = xt[tok]
        a = jax.nn.silu(xb @ w_gate[e]) * (xb @ w_up[e])
        out = (a @ w_down[e]) * wt[:, None].astype(xt.dtype)
        return y.at[tok].add(out), None

    y, _ = lax.scan(block, jnp.zeros_like(xt),
                    (slot_tok.reshape(n_blocks, EXPERT_BLOCK),
                     slot_w.reshape(n_blocks, EXPERT_BLOCK), block_e))
    return y.reshape(b_, s, d)


def setup_inputs(seed: int = 0) -> dict:
    key = jax.random.key(seed)
    ks = iter(jax.random.split(key, 48))
    nrm = lambda shape, scale: jax.random.normal(next(ks), shape, jnp.float32) * scale
    dsc = D_MODEL ** -0.5
    beta = DEEPNORM_BETA
    col_scales = (dsc, dsc, dsc * beta, 0.1 * dsc,
                  dsc, dsc, dsc * beta, 0.1 * dsc, 0.1 * dsc, dsc,
                  dsc, dsc)
    w_in = jnp.concatenate([nrm((DEPTH, D_MODEL, w), sc)
                            for w, sc in zip(IN_SPLIT_WIDTHS, col_scales)], axis=-1)
    return {
        "x": nrm((BATCH, SEQ, D_MODEL), 1.0),
        "ln_in_g": 1.0 + nrm((D_MODEL,), 0.02),
        "ln_in_b": nrm((D_MODEL,), 0.02),
        "w_in": w_in,
        "b_fox_f": jax.random.uniform(next(ks), (DEPTH, FOX_HEADS), jnp.float32, 3.0, 6.0),
        "b_ml_i": nrm((DEPTH, ML_HEADS), 0.5) - 1.0,
        "b_ml_f": jax.random.uniform(next(ks), (DEPTH, ML_HEADS), jnp.float32, 3.0, 6.0),
        "conv_w": nrm((DEPTH, CONV_WIDTH, 2 * ML_QK_WIDTH), CONV_WIDTH ** -0.5),
        "conv_b": nrm((DEPTH, 2 * ML_QK_WIDTH), 0.02),
        "ml_norm_g": 1.0 + nrm((DEPTH, ML_V_WIDTH), 0.02),
        "w_proj_fox": nrm((DEPTH, FOX_WIDTH, D_MODEL), FOX_WIDTH ** -0.5 * beta),
        "w_proj_ml": nrm((DEPTH, ML_V_WIDTH, D_MODEL), ML_V_WIDTH ** -0.5 * beta),
        "w_out": nrm((DEPTH, D_MODEL, D_MODEL), dsc * beta),
        "ln_mix_g": 1.0 + nrm((DEPTH, D_MODEL), 0.02),
        "ln_mix_b": nrm((DEPTH, D_MODEL), 0.02),
        "w_group": nrm((DEPTH, D_MODEL, N_GROUPS), dsc),
        "b_group": nrm((DEPTH, N_GROUPS), 0.01),
        "w_router": nrm((DEPTH, D_MODEL, N_EXPERTS), dsc),
        "b_router": nrm((DEPTH, N_EXPERTS), 0.01),
        "w_gate": nrm((DEPTH, N_EXPERTS, D_MODEL, D_EXPERT), dsc),
        "w_up": nrm((DEPTH, N_EXPERTS, D_MODEL, D_EXPERT), dsc * beta),
        "w_down": nrm((DEPTH, N_EXPERTS, D_EXPERT, D_MODEL), D_EXPERT ** -0.5 * beta),
        "ln_moe_g": 1.0 + nrm((DEPTH, D_MODEL), 0.02),
        "ln_moe_b": nrm((DEPTH, D_MODEL), 0.02),
    }


def reference(x, ln_in_g, ln_in_b, w_in, b_fox_f, b_ml_i, b_ml_f, conv_w, conv_b, ml_norm_g,
              w_proj_fox, w_proj_ml, w_out, ln_mix_g, ln_mix_b, w_group, b_group, w_router,
              b_router, w_gate, w_up, w_down, ln_moe_g, ln_moe_b):
    h = layer_norm(x, ln_in_g, ln_in_b)
    for l in range(DEPTH):
        mix = hybrid_mixer(h, w_in[l], b_fox_f[l], b_ml_i[l], b_ml_f[l], conv_w[l], conv_b[l],
                           ml_norm_g[l], w_proj_fox[l], w_proj_ml[l], w_out[l])
        h = layer_norm(DEEPNORM_ALPHA * h + mix, ln_mix_g[l], ln_mix_b[l])
        ffn = hierarchical_moe(h, w_group[l], b_group[l], w_router[l], b_router[l],
                               w_gate[l], w_up[l], w_down[l])
        h = layer_norm(DEEPNORM_ALPHA * h + ffn, ln_moe_g[l], ln_moe_b[l])
    return h
```

```python
import os
import numpy as np
import concourse.bass as bass
import concourse.mybir as mybir
from concourse.bass_utils import run_bass_kernel_spmd

F32 = mybir.dt.float32
BF16 = mybir.dt.bfloat16
I32 = mybir.dt.int32
AF = mybir.ActivationFunctionType
ALU = mybir.AluOpType
AX = mybir.AxisListType

PE, ACT, DVE, POOL, SP = "tensor", "scalar", "vector", "gpsimd", "sync"
ENGS = (PE, ACT, DVE, POOL, SP)
DMA_SLOTS = {SP: 24, POOL: 16, ACT: 8}


class Rec:
    __slots__ = ("eng", "fn", "deps", "kind", "signal", "event", "slotwait")

    def __init__(self, eng, fn, deps, kind):
        self.eng = eng
        self.fn = fn
        self.deps = deps
        self.kind = kind
        self.signal = kind != "c"
        self.event = None
        self.slotwait = None


class Prog:
    def __init__(self, nc):
        self.nc = nc
        self.ins = []
        self.last_w = {}
        self.readers = {}
        self.last_on = {}
        self.async_since_barrier = []
        self.persist = {}

    def op(self, eng, fn, r=(), w=(), dma=False, cc=False, extra=()):
        idx = len(self.ins)
        kind = "cc" if cc else ("d" if dma else "c")
        deps = set(extra)
        for k in r:
            lw = self.last_w.get(k)
            if lw is not None:
                deps.add(lw)
        for k in w:
            lw = self.last_w.get(k)
            if lw is not None:
                deps.add(lw)
            for rd in self.readers.get(k, ()):
                deps.add(rd)
        if eng == PE and kind == "c":
            deps = {d for d in deps if not (self.ins[d].eng == PE and self.ins[d].kind == "c")}
        rec = Rec(eng, fn, deps, kind)
        self.ins.append(rec)
        for d in deps:
            self.ins[d].signal = True
        for k in r:
            self.readers.setdefault(k, []).append(idx)
        for k in w:
            self.last_w[k] = idx
            self.readers[k] = []
        if kind == "cc":
            for k in w:
                self.persist[k] = idx
        else:
            self.last_on[eng] = idx
        if kind == "d":
            self.async_since_barrier.append(idx)
        return idx

    def barrier(self):
        pend = set(self.last_on.values()) | set(self.async_since_barrier)
        self.async_since_barrier = []
        for e in ENGS:
            self.op(e, None, extra=tuple(pend))
        self.last_w = dict(self.persist)
        self.readers = {}

    def finish(self, final_keys):
        self.op(SP, None, r=tuple(final_keys))

    def emit(self):
        nc = self.nc
        per = {e: [] for e in ENGS}
        for i, rec in enumerate(self.ins):
            per[rec.eng].append(i)
        csem = {e: nc.alloc_semaphore("c_" + e) for e in ENGS}
        dsem = {e: [nc.alloc_semaphore("d_%s_%d" % (e, i)) for i in range(n)] for e, n in DMA_SLOTS.items()}
        ccsem = nc.alloc_semaphore("ccs")
        ccount = {e: 0 for e in ENGS}
        dcount = {e: 0 for e in DMA_SLOTS}
        cccount = 0
        for rec in self.ins:
            if rec.kind == "d":
                n = DMA_SLOTS[rec.eng]
                i = dcount[rec.eng]
                dcount[rec.eng] += 1
                slot, use = i % n, i // n
                rec.event = (dsem[rec.eng][slot], 16 * (use + 1))
                rec.slotwait = (dsem[rec.eng][slot], 16 * use) if use > 0 else None
            elif rec.kind == "cc":
                cccount += 1
                rec.event = (ccsem, cccount)
            elif rec.signal:
                ccount[rec.eng] += 1
                rec.event = (csem[rec.eng], ccount[rec.eng])
        ins = self.ins

        def run(eng_name, e):
            waited = {}
            for i in per[eng_name]:
                rec = ins[i]
                for d in sorted(rec.deps):
                    sem, val = ins[d].event
                    if waited.get(sem.num, 0) >= val:
                        continue
                    e.wait_ge(sem, val)
                    waited[sem.num] = val
                if rec.slotwait is not None:
                    sem, val = rec.slotwait
                    if waited.get(sem.num, 0) < val:
                        e.wait_ge(sem, val)
                        waited[sem.num] = val
                if rec.fn is None:
                    if rec.signal:
                        e.nop().then_inc(rec.event[0], 1)
                    continue
                bi = rec.fn(e)
                if rec.kind == "d":
                    bi.then_inc(rec.event[0], 16)
                elif rec.kind == "cc":
                    bi.then_inc(rec.event[0])
                elif rec.signal:
                    bi.then_inc(rec.event[0], 1)

        with nc.Block() as block:
            @block.tensor
            def _(e):
                run(PE, e)

            @block.scalar
            def _(e):
                run(ACT, e)

            @block.vector
            def _(e):
                run(DVE, e)

            @block.gpsimd
            def _(e):
                run(POOL, e)

            @block.sync
            def _(e):
                run(SP, e)
        return {e: len(per[e]) for e in ENGS}


class Arena:
    def __init__(self, nc, kbytes_per_partition):
        self.ncols = kbytes_per_partition * 1024 // 4
        self.t = nc.alloc_sbuf_tensor("arena", [128, self.ncols], F32)
        self.off = 0
        self.marks = []

    def alloc(self, cols, dtype=F32, parts=128):
        esz = 4 if dtype in (F32, I32) else 2
        ncol32 = (cols * esz + 3) // 4
        ncol32 = (ncol32 + 7) // 8 * 8
        assert self.off + ncol32 <= self.ncols, "SBUF arena overflow %d + %d > %d" % (self.off, ncol32, self.ncols)
        ap = self.t[0:parts, self.off:self.off + ncol32]
        self.off += ncol32
        if dtype != F32:
            ap = ap.bitcast(dtype)
        return ap[:, 0:cols]

    def mark(self):
        self.marks.append(self.off)

    def release(self):
        self.off = self.marks.pop()


D = 4096
NCORE = 8
RANKS = 4
FH = 16
FD = 128
MLH = 4
MQK = 256
MV = 512
NE = 64
DE = 512
CAP = 128
EPS = 1e-5
ALPHA = 2.0 ** 0.25
CS = 64
KD = D // 128
O_FQ, O_FK, O_FV, O_FF = 0, 2048, 4096, 6144
O_MQ, O_MK, O_MV, O_MI, O_MF, O_MO = 6160, 7184, 8208, 10256, 10260, 10264
O_GA, O_GB = 12312, 16408


def build_program(S, debug=False, upto="ALL"):
    nc = bass.Bass("TRN2", target_bir_lowering=False)
    T = S // RANKS
    NT = S // 128
    GS = 512
    NG = S // GS
    NC = S // CS
    TGS = min(512, T)
    NTG = T // TGS
    TPG = TGS // 128
    NTT = T // 128
    SEGC = GS // CS

    in_names = []

    in_aps = []

    def din(name, shape, dt=F32):
        in_names.append(name)
        ap = nc.dram_tensor(name, shape, dt, kind="ExternalInput").ap()
        in_aps.append((name, ap))
        return ap

    LV = {"A": 1, "B": 2, "C": 3, "D3": 4, "D4": 5, "ALL": 6}[upto]
    lnp = din("lnp", [6, D])
    wa = din("wa", [D, 1536])
    wb = din("wb", [D, 1536])
    wg6 = din("wg6", [D, 6])
    gb6 = din("gb6", [6, 1])
    convw = din("convw", [128, 16])
    convb = din("convb", [128, 4])
    mlg = din("mlg", [1, 512])
    wr = din("wr", [D, 72])
    br = din("br", [1, 72])
    ytab = din("ytab", [128, 32 * NTG], I32)
    x_own = din("x_own", [T, D])
    out = nc.dram_tensor("out", [T, D], F32, kind="ExternalOutput").ap()

    dbg = {}
    KCUT = int(os.environ.get('KCUT', '99'))


    def scratch(name, shape, dt, dump=False):
        if debug and dump:
            ap = nc.dram_tensor(name, shape, dt, kind="ExternalOutput").ap()
            dbg[name] = ap
            return ap
        return nc.dram_tensor(name, shape, dt).ap()

    hT_d = scratch("hT_d", [D, S], BF16, dump=True)
    mqT_d = scratch("mqT_d", [512, S], BF16, dump=True)
    mv_d = scratch("mv_d", [S, 512], BF16, dump=True)
    mo_d = scratch("mo_d", [S, 512], BF16)
    y_d = scratch("y_d", [4096, T], BF16)
    yall2 = scratch("yall2", [RANKS * 4096 * NTG, TGS], BF16)
    yall_d = yall2.rearrange("(r g) t -> r (g t)", g=NTG)
    hTt_d = scratch("hTt_d", [D, T], BF16)
    h_d = scratch("h_d", [T, D], F32)
    mT_d = scratch("mT_d", [D, T], BF16, dump=True)
    y3 = y_d.rearrange("(c q) t -> c q t", q=4)
    YW = min(GS, T)
    h1_d = scratch("h1_d", [T, D], F32, dump=True)
    xg_d = scratch("xg_d", [NE * CAP, D], BF16)
    og_d = scratch("og_d", [NE * CAP, D], BF16)
    if debug:
        yall_dbg = scratch("yall_dbg", [RANKS * 4096, T], BF16, dump=True)
        gate_dbg = scratch("gate_dbg", [8, S], F32, dump=True)
        rout_dbg = scratch("rout_dbg", [T, 4], F32, dump=True)
        ffn_dbg = scratch("ffn_dbg", [T, D], F32, dump=True)

    A = Arena(nc, 204)
    P = Prog(nc)
    ps = [nc.alloc_psum_tensor("ps%d" % i, [128, 512], F32).ap() for i in range(8)]

    def psb(i):
        return ps[i].bitcast(BF16)

    ALL8 = [list(range(NCORE))]
    GRP4 = [[0, 1, 2, 3], [4, 5, 6, 7]]

    CCB = 512 * 1024

    def gathered(name, R, C, groups, nranks, esz=4):
        rows = R // nranks
        cr = max(1, CCB // (C * esz))
        assert rows % cr == 0
        K = rows // cr
        sh = din(name + "_s", [rows, C])
        bn = nc.dram_tensor(name + "_b", [rows, C], F32).ap()
        full = nc.dram_tensor(name + "_f", [R, C], F32).ap()
        crow = min(rows, max(128, (8 << 20) // (C * esz)))
        keys = []
        for r0 in range(0, rows, crow):
            P.op(SP, lambda e, r0=r0: e.dma_start(out=bn[r0:r0 + crow, :].rearrange("(p a) c -> p (a c)", p=128),
                                                  in_=sh[r0:r0 + crow, :].rearrange("(p a) c -> p (a c)", p=128)), w=[(name, "b", r0)], dma=True)
            keys.append((name, "b", r0))
        for k in range(K):
            P.op(POOL, lambda e, k=k: e.collective_compute("AllGather", ALU.bypass, replica_groups=groups, ins=[bn[k * cr:(k + 1) * cr, :].opt()],
                                                           outs=[full[k * nranks * cr:(k + 1) * nranks * cr, :].opt()]), r=keys, w=[name + "_f"], cc=True)
        return full

    x = din("x", [S, D])
    if LV >= 4:
        wgate = gathered("wgate", D, 2 * D, ALL8, NCORE)
        wpf = gathered("wpf", 2048, D, ALL8, NCORE)
        wpm = gathered("wpm", 2048, D, ALL8, NCORE)
        wo = gathered("wo", D, D, ALL8, NCORE)
    if LV >= 6:
        EPP = 16
        wegp = [gathered("weg%d" % p_, EPP * D, DE, ALL8, NCORE) for p_ in range(NE // EPP)]
        weup = [gathered("weu%d" % p_, EPP * D, DE, ALL8, NCORE) for p_ in range(NE // EPP)]
        wedp = [gathered("wed%d" % p_, EPP * DE, D, ALL8, NCORE) for p_ in range(NE // EPP)]

    ident_bf = A.alloc(128, BF16)
    ident_f = A.alloc(128)
    ones_bf = A.alloc(128, BF16)
    ones_f = A.alloc(128)
    mask64 = A.alloc(64)
    tri_s = A.alloc(128, BF16)
    fmask = [A.alloc(512, BF16) for _ in range(4)]
    zero_bf = A.alloc(2048, BF16)

    def sel(out_ap, pattern, op, base, cm, fill=0.0):
        return lambda e: e.affine_select(out=out_ap, in_=out_ap, pattern=pattern, compare_op=op, fill=fill, base=base,
                                         channel_multiplier=cm)

    P.op(POOL, lambda e: e.memset(ident_bf, 1.0), w=["ident_bf"])
    P.op(POOL, sel(ident_bf, [[-1, 128]], ALU.is_equal, 0, 1), r=["ident_bf"], w=["ident_bf"])
    P.op(POOL, lambda e: e.memset(ident_f, 1.0), w=["ident_f"])
    P.op(POOL, sel(ident_f, [[-1, 128]], ALU.is_equal, 0, 1), r=["ident_f"], w=["ident_f"])
    P.op(POOL, lambda e: e.memset(ones_bf, 1.0), w=["ones_bf"])
    P.op(POOL, lambda e: e.memset(ones_f, 1.0), w=["ones_f"])
    P.op(POOL, lambda e: e.memset(mask64, 1.0), w=["mask64"])
    P.op(POOL, sel(mask64[0:64, :], [[1, 64]], ALU.is_ge, 0, -1), r=["mask64"], w=["mask64"])
    P.op(POOL, sel(mask64[64:128, :], [[1, 64]], ALU.is_ge, 0, -1), r=["mask64"], w=["mask64"])
    P.op(POOL, lambda e: e.memset(tri_s, 1.0), w=["tri_s"])
    P.op(POOL, sel(tri_s, [[1, 128]], ALU.is_gt, 0, -1), r=["tri_s"], w=["tri_s"])
    for j in range(4):
        P.op(POOL, lambda e, j=j: e.memset(fmask[j], 1.0), w=[("fmask", j)])
        P.op(POOL, sel(fmask[j], [[1, 512]], ALU.is_ge, -128 * j, -1), r=[("fmask", j)], w=[("fmask", j)])
    P.op(POOL, lambda e: e.memset(zero_bf, 0.0), w=["zero_bf"])
    for i in range(NE * CAP // 128):
        for hcol in range(2):
            P.op(SP, lambda e, i=i, hcol=hcol: e.dma_start(out=xg_d[i * 128:(i + 1) * 128, hcol * 2048:(hcol + 1) * 2048], in_=zero_bf),
                 r=["zero_bf"], w=[("xg_d", i)], dma=True)
    CONST_KEYS = ["ident_bf", "ident_f", "ones_bf", "ones_f", "mask64", "tri_s", "zero_bf"] + [("fmask", j) for j in range(4)]

    touch = A.alloc(64, I32)
    for ti, (tn, tap) in enumerate(in_aps):
        src = tap
        while len(src.shape) > 1:
            src = src[0]
        v = src[0:1].bitcast(I32) if tap.dtype != I32 else src[0:1]
        P.op(SP, lambda e, ti=ti, v=v: e.dma_start(out=touch[0:1, ti:ti + 1], in_=v.unsqueeze(0)), w=[("touch", ti)], dma=True)
    if KCUT == 1:
        P.finish(list(dbg.keys()))
        return nc, P.emit(), list(dbg.keys()), in_names
    def layer_norm_tile(xt, gi, g_bc, b_bc, outs, tag, stats, mv, rstd):
        for k in range(8):
            P.op(DVE, lambda e, k=k: e.bn_stats(stats[:, k * 6:(k + 1) * 6], xt[:, k * 512:(k + 1) * 512]), r=[tag], w=[(tag, "st")])
        P.op(DVE, lambda e: e.bn_aggr(mv, stats), r=[(tag, "st")], w=[(tag, "mv")])
        P.op(DVE, lambda e: e.tensor_scalar_add(rstd, mv[:, 1:2], EPS), r=[(tag, "mv")], w=[(tag, "rs")])
        P.op(ACT, lambda e: e.sqrt(rstd, rstd), r=[(tag, "rs")], w=[(tag, "rs")])
        P.op(DVE, lambda e: e.reciprocal(rstd, rstd), r=[(tag, "rs")], w=[(tag, "rs")])
        P.op(DVE, lambda e: e.tensor_scalar(xt, xt, mv[:, 0:1], rstd, ALU.subtract, ALU.mult), r=[tag, (tag, "mv"), (tag, "rs")], w=[tag])
        P.op(POOL, lambda e: e.tensor_tensor(xt, xt, g_bc, ALU.mult), r=[tag, "lnbc"], w=[tag])
        for (o, ok) in outs:
            P.op(DVE, lambda e, o=o: e.tensor_tensor(o, xt, b_bc, ALU.add), r=[tag, "lnbc"], w=[ok])

    ctok = A.alloc(NT * 4)
    cref = A.alloc(NG * 4)
    u_tok = A.alloc(NC)
    e_tok = A.alloc(NC)
    dec_bc = A.alloc(NC + 1)
    def ln_phase(xsrc, ntiles, gsz, hT_dst, h_dst=None):
        A.mark()
        g_bc = A.alloc(D)
        b_bc = A.alloc(D)
        P.op(SP, lambda e: e.dma_start(out=g_bc, in_=lnp[0:1, :].to_broadcast([128, D])), w=["lnbc"], dma=True)
        P.op(SP, lambda e: e.dma_start(out=b_bc, in_=lnp[1:2, :].to_broadcast([128, D])), w=["lnbc"], dma=True)
        xts = [A.alloc(D) for _ in range(2)]
        hbs = [A.alloc(D, BF16) for _ in range(2)]
        hTg = [A.alloc(KD * gsz, BF16) for _ in range(2)]
        stats = A.alloc(48)
        mvt = A.alloc(2)
        rstd = A.alloc(1)
        tpg = gsz // 128
        for i in range(ntiles):
            g, j = i // tpg, i % tpg
            xt = xts[i % 2]
            hb = hbs[i % 2]
            P.op(SP, lambda e, xt=xt, i=i: e.dma_start(out=xt, in_=xsrc[i * 128:(i + 1) * 128, :]), w=[("xt", i % 2)], dma=True)
            if h_dst is None:
                layer_norm_tile(xt, i, g_bc, b_bc, [(hb, ("hb", i % 2))], ("xt", i % 2), stats, mvt, rstd)
            else:
                layer_norm_tile(xt, i, g_bc, b_bc, [(xt, ("xt", i % 2))], ("xt", i % 2), stats, mvt, rstd)
                P.op(ACT, lambda e, hb=hb, xt=xt: e.copy(hb, xt), r=[("xt", i % 2)], w=[("hb", i % 2)])
                P.op(SP, lambda e, xt=xt, i=i: e.dma_start(out=h_dst[i * 128:(i + 1) * 128, :], in_=xt), r=[("xt", i % 2)], w=["h_dst"], dma=True)
            hv = hTg[g % 2].rearrange("p (c t) -> p c t", c=KD)
            for q in range(4):
                for cc in range(8):
                    c = q * 8 + cc
                    P.op(PE, lambda e, q=q, cc=cc, c=c, hb=hb: e.transpose(psb(q)[:, cc * 128:(cc + 1) * 128], hb[:, c * 128:(c + 1) * 128], ident_bf),
                         r=[("hb", i % 2), "ident_bf"], w=[("ps", q)])
                P.op(ACT, lambda e, q=q, hv=hv, j=j: e.copy(hv[:, q * 8:(q + 1) * 8, j * 128:(j + 1) * 128],
                                                           psb(q).rearrange("p (c t) -> p c t", c=8)),
                     r=[("ps", q)], w=[("hTg", g % 2)])
            if j == tpg - 1:
                P.op(SP, lambda e, hv=hv, g=g: e.dma_start(out=hT_dst[:, g * gsz:(g + 1) * gsz].rearrange("(c p) t -> p c t", p=128), in_=hv),
                     r=[("hTg", g % 2)], w=["hT_dst"], dma=True)
        P.barrier()
        A.release()

    ln_phase(x, NT, GS, hT_d)

    if KCUT == 3:
        P.finish(list(dbg.keys()))
        return nc, P.emit(), list(dbg.keys()), in_names
    A.mark()
    NPB = 2
    G6 = A.alloc(S)
    gb6t = A.alloc(1)
    A.mark()
    hbuf = [A.alloc(KD * GS, BF16) for _ in range(NPB)]

    def load_hT(g, slot):
        hv = hbuf[slot].rearrange("p (c t) -> p c t", c=KD)
        P.op(SP, lambda e: e.dma_start(out=hv, in_=hT_d[:, g * GS:(g + 1) * GS].rearrange("(c p) t -> p c t", p=128)),
             r=["hT_d"], w=[("hbuf", slot)], dma=True)
        return hv

    P.op(SP, lambda e: e.dma_start(out=gb6t[0:6, :], in_=gb6), w=["gb6t"], dma=True)
    A.mark()
    w1 = A.alloc(KD * 512, BF16)
    w1v = w1.rearrange("p (c n) -> p c n", c=KD)
    P.op(POOL, lambda e: e.dma_start(out=w1v, in_=wa[:, 1024:1536].rearrange("(c p) n -> p c n", p=128)), w=["w1"], dma=True)
    w6f = A.alloc(KD * 6)
    w6fv = w6f.rearrange("p (c n) -> p c n", c=KD)
    P.op(SP, lambda e: e.dma_start(out=w6fv, in_=wg6.rearrange("(c p) n -> p c n", p=128)), w=["w6f"], dma=True)
    w6 = A.alloc(KD * 6, BF16)
    P.op(DVE, lambda e: e.tensor_copy(w6, w6f), r=["w6f"], w=["w6"])
    w6v = w6.rearrange("p (c n) -> p c n", c=KD)
    cw = A.alloc(16)
    cbias = A.alloc(4)
    P.op(SP, lambda e: e.dma_start(out=cw, in_=convw), w=["cw"], dma=True)
    P.op(SP, lambda e: e.dma_start(out=cbias, in_=convb), w=["cw"], dma=True)
    xpre = [A.alloc(GS + 3) for _ in range(4)]
    cacc = A.alloc(GS)
    qko = [A.alloc(4 * GS, BF16) for _ in range(2)]
    for a in range(4):
        P.op(POOL, lambda e, a=a: e.memset(xpre[a][:, 0:3], 0.0), w=[("xpre", a)])
    for g in range(NG):
        hv = load_hT(g, g % NPB)
        qv = qko[g % 2].rearrange("p (a t) -> p a t", a=4)
        for a in range(4):
            bank = a % 2
            for c in range(KD):
                P.op(PE, lambda e, a=a, c=c, hv=hv, bank=bank: e.matmul(ps[bank], w1v[:, c, a * 128:(a + 1) * 128], hv[:, c, :], start=(c == 0), stop=(c == KD - 1)),
                     r=["w1", ("hbuf", g % NPB)], w=[("ps", bank)])
            P.op(ACT, lambda e, a=a, bank=bank: e.copy(xpre[a][:, 3:3 + GS], ps[bank]), r=[("ps", bank)], w=[("xpre", a)])
            P.op(DVE, lambda e, a=a: e.tensor_scalar(cacc, xpre[a][:, 0:GS], cw[:, a * 4:a * 4 + 1], None, ALU.mult), r=[("xpre", a), "cw"], w=["cacc"])
            for jj in range(1, 4):
                P.op(DVE, lambda e, a=a, jj=jj: e.scalar_tensor_tensor(cacc, xpre[a][:, jj:jj + GS], cw[:, a * 4 + jj:a * 4 + jj + 1], cacc, ALU.mult, ALU.add),
                     r=[("xpre", a), "cw", "cacc"], w=["cacc"])
            P.op(POOL, lambda e, a=a: e.tensor_copy(xpre[a][:, 0:3], xpre[a][:, GS:GS + 3]), r=[("xpre", a)], w=[("xpre", a)])
            if a < 2:
                P.op(ACT, lambda e, a=a, qv=qv: e.activation(qv[:, a, :], cacc, AF.Silu, bias=cbias[:, a:a + 1]), r=["cacc", "cw"], w=[("qko", g % 2)])
            else:
                P.op(ACT, lambda e, a=a: e.activation(cacc, cacc, AF.Silu, bias=cbias[:, a:a + 1]), r=["cacc", "cw"], w=["cacc"])
                P.op(POOL, lambda e, a=a, qv=qv: e.tensor_scalar(qv[:, a, :], cacc, MQK ** -0.5, None, ALU.mult), r=["cacc"], w=[("qko", g % 2)])
        for c in range(KD):
            P.op(PE, lambda e, c=c, hv=hv: e.matmul(ps[2][0:6, :], w6v[:, c, :], hv[:, c, :], start=(c == 0), stop=(c == KD - 1)),
                 r=["w6", ("hbuf", g % NPB)], w=[("ps", 2)])
        P.op(ACT, lambda e, g=g: e.copy(G6[0:6, g * GS:(g + 1) * GS], ps[2][0:6, :]), r=[("ps", 2)], w=["G6"])
        P.op(SP, lambda e, g=g, qv=qv: e.dma_start(out=mqT_d[:, g * GS:(g + 1) * GS].rearrange("(a p) t -> p a t", p=128), in_=qv),
             r=[("qko", g % 2)], w=["mqT_d"], dma=True)
    P.barrier()
    A.release()
    if KCUT == 4:
        P.finish(list(dbg.keys()))
        return nc, P.emit(), list(dbg.keys()), in_names
    A.mark()
    w2 = A.alloc(KD * 1024, BF16)
    w2v = w2.rearrange("p (c n) -> p c n", c=KD)
    P.op(POOL, lambda e: e.dma_start(out=w2v, in_=wb[:, 512:1536].rearrange("(c p) n -> p c n", p=128)), w=["w2"], dma=True)
    vo = [A.alloc(1024, BF16) for _ in range(2)]
    for g in range(NG):
        hv = load_hT(g, g % NPB)
        for j in range(4):
            i = g * 4 + j
            vt = vo[i % 2]
            for half in range(2):
                bank = half
                for c in range(KD):
                    P.op(PE, lambda e, c=c, half=half, hv=hv, j=j, bank=bank: e.matmul(ps[bank], hv[:, c, j * 128:(j + 1) * 128], w2v[:, c, half * 512:(half + 1) * 512],
                                                                                      start=(c == 0), stop=(c == KD - 1)),
                         r=["w2", ("hbuf", g % NPB)], w=[("ps", bank)])
            P.op(ACT, lambda e, vt=vt: e.copy(vt[:, 0:512], ps[0]), r=[("ps", 0)], w=[("vo", i % 2)])
            P.op(ACT, lambda e, vt=vt: e.activation(vt[:, 512:1024], ps[1], AF.Sigmoid), r=[("ps", 1)], w=[("vo", i % 2)])
            P.op(SP, lambda e, vt=vt, i=i: e.dma_start(out=mv_d[i * 128:(i + 1) * 128, :], in_=vt[:, 0:512]), r=[("vo", i % 2)], w=["mv_d"], dma=True)
            P.op(SP, lambda e, vt=vt, i=i: e.dma_start(out=mo_d[i * 128:(i + 1) * 128, :], in_=vt[:, 512:1024]), r=[("vo", i % 2)], w=["mo_d"], dma=True)
    P.barrier()
    A.release()

    if KCUT == 5:
        P.finish(list(dbg.keys()))
        return nc, P.emit(), list(dbg.keys()), in_names
    A.release()
    ngb = A.alloc(1)
    P.op(DVE, lambda e: e.tensor_scalar(ngb[0:6, :], gb6t[0:6, :], -1.0, None, ALU.mult), r=["gb6t"], w=["ngb"])
    LF = G6
    P.op(ACT, lambda e: e.activation(LF[0:5, :], G6[0:5, :], AF.Exp, bias=ngb[0:5, :], scale=-1.0), r=["G6", "ngb"], w=["LF"])
    P.op(ACT, lambda e: e.activation(LF[0:5, :], LF[0:5, :], AF.Ln, bias=1.0, scale=1.0), r=["LF"], w=["LF"])
    P.op(DVE, lambda e: e.tensor_scalar(LF[0:5, :], LF[0:5, :], -1.0, None, ALU.mult), r=["LF"], w=["LF"])
    CUM = A.alloc(S)
    SCN = 512
    A.mark()
    Bm = A.alloc(S)
    MI = A.alloc(S)
    TMP = A.alloc(S)
    ones5 = A.alloc(512)
    gbi = A.alloc(1)
    crefb = A.alloc(NG * 128)
    P.op(POOL, lambda e: e.memset(ones5[0:5, :], 1.0), w=["ones5"])
    P.op(SP, lambda e: e.dma_start(out=gbi[0:1, :], in_=gb6[5:6, :]), w=["gbi"], dma=True)
    for k in range(S // SCN):
        init = 0.0 if k == 0 else CUM[0:5, k * SCN - 1:k * SCN]
        P.op(DVE, lambda e, k=k, init=init: e.tensor_tensor_scan(CUM[0:5, k * SCN:(k + 1) * SCN], ones5[0:5, :],
                                                                LF[0:5, k * SCN:(k + 1) * SCN], init, ALU.mult, ALU.add),
             r=["LF", "ones5", "CUM"], w=["CUM"])
    P.op(SP, lambda e: e.dma_start(out=Bm[0:1, :], in_=CUM[4:5, :]), r=["CUM"], w=["Bm"], dma=True)
    P.op(SP, lambda e: e.dma_start(out=MI[0:1, :], in_=G6[5:6, :]), r=["G6"], w=["MI"], dma=True)
    if debug:
        P.op(SP, lambda e: e.dma_start(out=gate_dbg[0:5, :], in_=CUM[0:5, :]), r=["CUM"], w=["gate_dbg"], dma=True)
    P.op(DVE, lambda e: e.scalar_tensor_tensor(MI[0:1, :], MI[0:1, :], gbi[0:1, :], Bm[0:1, :], ALU.add, ALU.subtract), r=["MI", "Bm", "gbi"], w=["MI"])
    for k in range(S // SCN):
        init = 0.0 if k == 0 else TMP[0:1, k * SCN - 1:k * SCN]
        P.op(DVE, lambda e, k=k, init=init: e.tensor_tensor_scan(TMP[0:1, k * SCN:(k + 1) * SCN], MI[0:1, k * SCN:(k + 1) * SCN],
                                                                MI[0:1, k * SCN:(k + 1) * SCN], init, ALU.max, ALU.max),
             r=["MI", "TMP"], w=["TMP"])
    AP_ = LF
    APv = AP_[0:1, :].rearrange("p (c l) -> p c l", l=CS)
    TMPv = TMP[0:1, :].rearrange("p (c l) -> p c l", l=CS)
    P.op(DVE, lambda e: e.memset(APv[:, 0, :], 0.0), r=["CUM"], w=["LF"])
    P.op(DVE, lambda e: e.tensor_copy(APv[:, 1:NC, :], TMPv[:, 0:NC - 1, CS - 1:CS].to_broadcast([1, NC - 1, CS])), r=["TMP"], w=["LF"])
    P.op(DVE, lambda e: e.tensor_tensor(MI[0:1, :], MI[0:1, :], AP_[0:1, :], ALU.subtract), r=["MI", "LF"], w=["MI"])
    P.op(ACT, lambda e: e.activation(MI[0:1, :], MI[0:1, :], AF.Exp), r=["MI"], w=["MI"])
    P.op(DVE, lambda e: e.tensor_tensor(TMP[0:1, :], AP_[0:1, :], TMP[0:1, :], ALU.subtract), r=["TMP", "LF"], w=["TMP"])
    P.op(ACT, lambda e: e.activation(TMP[0:1, :], TMP[0:1, :], AF.Exp), r=["TMP"], w=["TMP"])
    P.op(DVE, lambda e: e.tensor_tensor(Bm[0:1, :], Bm[0:1, :], AP_[0:1, :], ALU.add), r=["Bm", "LF"], w=["Bm"])
    P.op(ACT, lambda e: e.activation(Bm[0:1, :], Bm[0:1, :], AF.Exp, scale=-1.0), r=["Bm"], w=["Bm"])
    if debug:
        P.op(SP, lambda e: e.dma_start(out=gate_dbg[5:6, :], in_=MI[0:1, :]), r=["MI"], w=["gate_dbg"], dma=True)
        P.op(SP, lambda e: e.dma_start(out=gate_dbg[6:7, :], in_=TMP[0:1, :]), r=["TMP"], w=["gate_dbg"], dma=True)
        P.op(SP, lambda e: e.dma_start(out=gate_dbg[7:8, :], in_=Bm[0:1, :]), r=["Bm"], w=["gate_dbg"], dma=True)
    for j in range(NC):
        P.op(PE, lambda e, j=j: e.matmul(ps[0][0:64, j:j + 1], MI[0:1, j * CS:(j + 1) * CS], ones_f[0:1, 0:1], start=True, stop=True),
             r=["MI", "ones_f"], w=[("ps", 0)])
        P.op(PE, lambda e, j=j: e.matmul(ps[1][0:64, j:j + 1], Bm[0:1, j * CS:(j + 1) * CS], ones_f[0:1, 0:1], start=True, stop=True),
             r=["Bm", "ones_f"], w=[("ps", 1)])
    P.op(ACT, lambda e: e.copy(u_tok[0:64, :], ps[0][0:64, 0:NC]), r=[("ps", 0)], w=["u_tok"])
    P.op(ACT, lambda e: e.copy(e_tok[0:64, :], ps[1][0:64, 0:NC]), r=[("ps", 1)], w=["e_tok"])
    P.op(PE, lambda e: e.matmul(ps[2][:, 0:NC], ones_f[0:1, :], TMP[0:1, CS - 1::CS], start=True, stop=True), r=["TMP", "ones_f"], w=[("ps", 2)])
    P.op(DVE, lambda e: e.memset(dec_bc[:, 0:1], 1.0), w=["dec_bc"])
    P.op(ACT, lambda e: e.copy(dec_bc[:, 1:NC + 1], ps[2][:, 0:NC]), r=[("ps", 2)], w=["dec_bc"])
    for i in range(NT):
        P.op(PE, lambda e, i=i: e.matmul(ps[3][:, 4 * i:4 * i + 4], CUM[0:4, i * 128:(i + 1) * 128], ident_f[0:4, 0:4], start=True, stop=True),
             r=["CUM", "ident_f"], w=[("ps", 3)])
    P.op(ACT, lambda e: e.copy(ctok, ps[3][:, 0:NT * 4]), r=[("ps", 3)], w=["ctok"])
    crefv = crefb[0:4, :].rearrange("p (g t) -> p g t", g=NG)
    P.op(DVE, lambda e: e.tensor_copy(crefv, CUM[0:4, GS - 1::GS].unsqueeze(2).to_broadcast([4, NG, 128])), r=["CUM"], w=["crefb"])
    for g in range(NG):
        P.op(PE, lambda e, g=g: e.matmul(ps[4][:, 4 * g:4 * g + 4], crefv[:, g, :], ident_f[0:4, 0:4], start=True, stop=True),
             r=["crefb", "ident_f"], w=[("ps", 4)])
    P.op(ACT, lambda e: e.copy(cref, ps[4][:, 0:NG * 4]), r=[("ps", 4)], w=["cref"])
    P.barrier()
    A.release()
    A.release()

    if LV < 2:
        P.finish(list(dbg.keys()))
        return nc, P.emit(), list(dbg.keys()), in_names
    A.mark()
    hbuf = [A.alloc(KD * GS, BF16) for _ in range(NPB)]
    wf = A.alloc(KD * 384, BF16)
    wfv = wf.rearrange("p (c n) -> p c n", c=KD)
    qT = A.alloc(S, BF16)
    kT = A.alloc(S, BF16)
    vt = A.alloc(NT * 128, BF16)
    biasH = A.alloc(NG * NT)
    PT = [A.alloc(512, BF16) for _ in range(4)]
    rden = A.alloc(512)
    ystage = [A.alloc(512, BF16) for _ in range(2)]
    ctv = ctok.rearrange("p (i h) -> p i h", h=4)
    for h in range(4):
        P.op(POOL, lambda e, h=h: e.dma_start(out=wfv[:, :, 0:128], in_=wa[:, h * 128:(h + 1) * 128].rearrange("(c p) n -> p c n", p=128)), w=["wf"], dma=True)
        P.op(POOL, lambda e, h=h: e.dma_start(out=wfv[:, :, 128:256], in_=wa[:, 512 + h * 128:512 + (h + 1) * 128].rearrange("(c p) n -> p c n", p=128)), w=["wf"], dma=True)
        P.op(POOL, lambda e, h=h: e.dma_start(out=wfv[:, :, 256:384], in_=wb[:, h * 128:(h + 1) * 128].rearrange("(c p) n -> p c n", p=128)), w=["wf"], dma=True)
        for g in range(NG):
            slot = g % NPB
            hv = load_hT(g, slot)
            for c in range(KD):
                P.op(PE, lambda e, c=c, hv=hv: e.matmul(ps[0], wfv[:, c, 0:128], hv[:, c, :], start=(c == 0), stop=(c == KD - 1)), r=["wf", ("hbuf", slot)], w=[("ps", 0)])
            P.op(ACT, lambda e, g=g: e.mul(qT[:, g * GS:(g + 1) * GS], ps[0], FD ** -0.5), r=[("ps", 0)], w=["qT"])
            for c in range(KD):
                P.op(PE, lambda e, c=c, hv=hv: e.matmul(ps[1], wfv[:, c, 128:256], hv[:, c, :], start=(c == 0), stop=(c == KD - 1)), r=["wf", ("hbuf", slot)], w=[("ps", 1)])
            P.op(ACT, lambda e, g=g: e.copy(kT[:, g * GS:(g + 1) * GS], ps[1]), r=[("ps", 1)], w=["kT"])
            for j in range(4):
                for c in range(KD):
                    P.op(PE, lambda e, c=c, j=j, hv=hv: e.matmul(ps[2][:, j * 128:(j + 1) * 128], hv[:, c, j * 128:(j + 1) * 128], wfv[:, c, 256:384], start=(c == 0), stop=(c == KD - 1)),
                         r=["wf", ("hbuf", slot)], w=[("ps", 2)])
            P.op(ACT, lambda e, g=g: e.copy(vt[:, g * 512:(g + 1) * 512], ps[2]), r=[("ps", 2)], w=["vt"])
        for g in range(NG):
            P.op(DVE, lambda e, g=g, h=h: e.tensor_scalar(biasH[:, g * NT:(g + 1) * NT], ctv[:, :, h], cref[:, g * 4 + h:g * 4 + h + 1], -1.0, ALU.subtract, ALU.mult),
                 r=["ctok", "cref"], w=["biasH"])
        for g in range(NG):
            ns = 4 * g + 4
            num = ps[4 + (g % 2) * 2]
            den = ps[5 + (g % 2) * 2]
            kn, kd = ("ps", 4 + (g % 2) * 2), ("ps", 5 + (g % 2) * 2)
            for s in range(ns):
                sb = s % 3
                pt = PT[s % 4]
                P.op(PE, lambda e, s=s, g=g, sb=sb: e.matmul(ps[sb], kT[:, s * 128:(s + 1) * 128], qT[:, g * GS:(g + 1) * GS], start=True, stop=True), r=["kT", "qT"], w=[("ps", sb)])
                P.op(ACT, lambda e, s=s, g=g, sb=sb, pt=pt: e.activation(pt, ps[sb], AF.Exp, bias=biasH[:, g * NT + s:g * NT + s + 1], scale=1.0), r=[("ps", sb), "biasH"], w=[("PT", s % 4)])
                if s >= 4 * g:
                    P.op(POOL, lambda e, pt=pt, j=s - 4 * g: e.tensor_tensor(pt, pt, fmask[j], ALU.mult), r=[("PT", s % 4), ("fmask", s - 4 * g)], w=[("PT", s % 4)])
                P.op(PE, lambda e, s=s, pt=pt, num=num, ns=ns: e.matmul(num, vt[:, s * 128:(s + 1) * 128], pt, start=(s == 0), stop=(s == ns - 1)), r=["vt", ("PT", s % 4)], w=[kn])
                P.op(PE, lambda e, s=s, pt=pt, den=den, ns=ns: e.matmul(den, ones_bf, pt, start=(s == 0), stop=(s == ns - 1)), r=["ones_bf", ("PT", s % 4)], w=[kd])
            ys = ystage[g % 2]
            P.op(DVE, lambda e, den=den: e.reciprocal(rden, den), r=[kd], w=["rden"])
            P.op(DVE, lambda e, num=num, ys=ys: e.tensor_tensor(ys, num, rden, ALU.mult), r=[kn, "rden"], w=[("ystage", g % 2)])
            for pc in range(GS // YW):
                tok0 = g * GS + pc * YW
                P.op(SP, lambda e, ys=ys, h=h, pc=pc, q=tok0 // T, tl=tok0 % T: e.dma_start(out=y3[h * 128:(h + 1) * 128, q, tl:tl + YW], in_=ys[:, pc * YW:(pc + 1) * YW]),
                     r=[("ystage", g % 2)], w=["y_d"], dma=True)
    P.barrier()
    A.release()

    A.mark()
    qk = A.alloc(4 * S, BF16)
    qkv = qk.rearrange("p (a t) -> p a t", a=4)
    P.op(SP, lambda e: e.dma_start(out=qkv, in_=mqT_d.rearrange("(a p) t -> p a t", p=128)), r=["mqT_d"], w=["qk"], dma=True)
    vseg = [A.alloc(SEGC * 512, BF16) for _ in range(2)]
    oseg = [A.alloc(SEGC * 512, BF16) for _ in range(2)]
    X = A.alloc(1024)
    Cb = [A.alloc(1024, BF16) for _ in range(2)]
    xn = A.alloc(2)
    nb = [A.alloc(2, BF16) for _ in range(2)]
    STs = [A.alloc(64, BF16) for _ in range(2)]
    kk = [A.alloc(256, BF16) for _ in range(2)]
    hrow = [A.alloc(512) for _ in range(2)]
    mlg_bc = A.alloc(512)
    yml = [A.alloc(512, BF16) for _ in range(2)]
    ymlT = [A.alloc(4 * GS, BF16) for _ in range(2)]
    dnm = A.alloc(1)
    rd = A.alloc(1)
    st6 = A.alloc(6)
    mv2 = A.alloc(2)
    rs2 = A.alloc(1)
    P.op(SP, lambda e: e.dma_start(out=mlg_bc[0:64, :], in_=mlg.to_broadcast([64, 512])), w=["mlg_bc"], dma=True)
    P.op(DVE, lambda e: e.memset(X, 0.0), w=["X"])
    P.op(DVE, lambda e: e.memset(xn, 0.0), w=["xn"])
    P.op(POOL, lambda e: e.memset(Cb[0], 0.0), w=[("Cb", 0)])
    P.op(POOL, lambda e: e.memset(nb[0], 0.0), w=[("nb", 0)])
    for j in range(NC):
        seg, l = j // SEGC, j % SEGC
        sl = seg % 2
        if l == 0:
            P.op(SP, lambda e, seg=seg, sl=sl: e.dma_start(out=vseg[sl][0:64, :].rearrange("p (c n) -> p c n", c=SEGC),
                                                          in_=mv_d[seg * GS:(seg + 1) * GS, :].rearrange("(c p) n -> p c n", p=64)), r=["mv_d"], w=[("vseg", sl)], dma=True)
            P.op(SP, lambda e, seg=seg, sl=sl: e.dma_start(out=oseg[sl][0:64, :].rearrange("p (c n) -> p c n", c=SEGC),
                                                          in_=mo_d[seg * GS:(seg + 1) * GS, :].rearrange("(c p) n -> p c n", p=64)), r=["mo_d"], w=[("oseg", sl)], dma=True)
        q0, q1 = qkv[:, 0, j * CS:(j + 1) * CS], qkv[:, 1, j * CS:(j + 1) * CS]
        k0, k1 = qkv[:, 2, j * CS:(j + 1) * CS], qkv[:, 3, j * CS:(j + 1) * CS]
        vj = vseg[sl][0:64, l * 512:(l + 1) * 512]
        oj = oseg[sl][0:64, l * 512:(l + 1) * 512]
        uj = u_tok[0:64, j:j + 1]
        b2 = j % 2
        st = ps[0][0:64, 0:64]
        kkp = psb(1)[0:64, 0:256]
        P.op(PE, lambda e, k0=k0, q0=q0, st=st: e.matmul(st, k0, q0, start=True, stop=False), r=["qk"], w=[("ps", 0)])
        P.op(PE, lambda e, k1=k1, q1=q1, st=st: e.matmul(st, k1, q1, start=False, stop=True), r=["qk"], w=[("ps", 0)])
        P.op(PE, lambda e, k0=k0, kkp=kkp: e.transpose(kkp[:, 0:128], k0, ident_bf), r=["qk", "ident_bf"], w=[("ps", 1)])
        P.op(PE, lambda e, k1=k1, kkp=kkp: e.transpose(kkp[:, 128:256], k1, ident_bf), r=["qk", "ident_bf"], w=[("ps", 1)])
        sts = STs[b2][0:64, :]
        P.op(DVE, lambda e, st=st, sts=sts, uj=uj: e.scalar_tensor_tensor(sts, st, uj, mask64[0:64, :], ALU.mult, ALU.mult), r=[("ps", 0), "u_tok", "mask64"], w=[("STs", b2)])
        kkj = kk[b2][0:64, :]
        P.op(ACT, lambda e, kkj=kkj, kkp=kkp, uj=uj: e.mul(kkj, kkp, uj), r=[("ps", 1), "u_tok"], w=[("kk", b2)])
        num = ps[2 + b2][0:64, :]
        den = ps[4][0:64, b2:b2 + 1]
        P.op(PE, lambda e, num=num, sts=sts, vj=vj: e.matmul(num, sts, vj, start=True, stop=False), r=[("STs", b2), ("vseg", sl)], w=[("ps", 2 + b2)])
        P.op(PE, lambda e, num=num, q0=q0, b2=b2: e.matmul(num, q0, Cb[b2][:, 0:512], start=False, stop=False), r=["qk", ("Cb", b2)], w=[("ps", 2 + b2)])
        P.op(PE, lambda e, num=num, q1=q1, b2=b2: e.matmul(num, q1, Cb[b2][:, 512:1024], start=False, stop=True), r=["qk", ("Cb", b2)], w=[("ps", 2 + b2)])
        P.op(PE, lambda e, den=den, sts=sts: e.matmul(den, sts, ones_bf[0:64, 0:1], start=True, stop=False), r=[("STs", b2), "ones_bf"], w=[("ps", 4)])
        P.op(PE, lambda e, den=den, q0=q0, b2=b2: e.matmul(den, q0, nb[b2][:, 0:1], start=False, stop=False), r=["qk", ("nb", b2)], w=[("ps", 4)])
        P.op(PE, lambda e, den=den, q1=q1, b2=b2: e.matmul(den, q1, nb[b2][:, 1:2], start=False, stop=True), r=["qk", ("nb", b2)], w=[("ps", 4)])
        P.op(PE, lambda e, kkj=kkj, vj=vj: e.matmul(ps[5], kkj[:, 0:128], vj, start=True, stop=True), r=[("kk", b2), ("vseg", sl)], w=[("ps", 5)])
        P.op(PE, lambda e, kkj=kkj, vj=vj: e.matmul(ps[6], kkj[:, 128:256], vj, start=True, stop=True), r=[("kk", b2), ("vseg", sl)], w=[("ps", 6)])
        P.op(PE, lambda e, kkj=kkj: e.matmul(ps[7][:, 0:1], kkj[:, 0:128], ones_bf[0:64, 0:1], start=True, stop=True), r=[("kk", b2), "ones_bf"], w=[("ps", 7)])
        P.op(PE, lambda e, kkj=kkj: e.matmul(ps[7][:, 1:2], kkj[:, 128:256], ones_bf[0:64, 0:1], start=True, stop=True), r=[("kk", b2), "ones_bf"], w=[("ps", 7)])
        dprev = dec_bc[:, j:j + 1]
        dcur = dec_bc[:, j + 1:j + 2]
        P.op(DVE, lambda e, dprev=dprev: e.scalar_tensor_tensor(X[:, 0:512], X[:, 0:512], dprev, ps[5], ALU.mult, ALU.add), r=["X", "dec_bc", ("ps", 5)], w=["X"])
        P.op(DVE, lambda e, dprev=dprev: e.scalar_tensor_tensor(X[:, 512:1024], X[:, 512:1024], dprev, ps[6], ALU.mult, ALU.add), r=["X", "dec_bc", ("ps", 6)], w=["X"])
        P.op(DVE, lambda e, dprev=dprev: e.scalar_tensor_tensor(xn, xn, dprev, ps[7][:, 0:2], ALU.mult, ALU.add), r=["xn", "dec_bc", ("ps", 7)], w=["xn"])
        nb2 = (j + 1) % 2
        P.op(ACT, lambda e, dcur=dcur, nb2=nb2: e.mul(Cb[nb2], X, dcur), r=["X", "dec_bc"], w=[("Cb", nb2)])
        P.op(ACT, lambda e, dcur=dcur, nb2=nb2: e.mul(nb[nb2], xn, dcur), r=["xn", "dec_bc"], w=[("nb", nb2)])
        hr = hrow[b2][0:64, :]
        P.op(ACT, lambda e, den=den: e.activation(dnm[0:64, :], den, AF.Abs), r=[("ps", 4)], w=["dnm"])
        P.op(DVE, lambda e, j=j: e.tensor_scalar(dnm[0:64, :], dnm[0:64, :], e_tok[0:64, j:j + 1], None, ALU.max), r=["dnm", "e_tok"], w=["dnm"])
        P.op(DVE, lambda e: e.reciprocal(rd[0:64, :], dnm[0:64, :]), r=["dnm"], w=["rd"])
        P.op(ACT, lambda e, hr=hr, num=num: e.mul(hr, num, rd[0:64, :]), r=[("ps", 2 + b2), "rd"], w=[("hrow", b2)])
        P.op(DVE, lambda e, hr=hr: e.bn_stats(st6[0:64, :], hr), r=[("hrow", b2)], w=["st6"])
        P.op(DVE, lambda e: e.bn_aggr(mv2[0:64, :], st6[0:64, :]), r=["st6"], w=["mv2"])
        P.op(DVE, lambda e: e.tensor_scalar_add(rs2[0:64, :], mv2[0:64, 1:2], EPS), r=["mv2"], w=["rs2"])
        P.op(ACT, lambda e: e.sqrt(rs2[0:64, :], rs2[0:64, :]), r=["rs2"], w=["rs2"])
        P.op(DVE, lambda e: e.reciprocal(rs2[0:64, :], rs2[0:64, :]), r=["rs2"], w=["rs2"])
        P.op(DVE, lambda e, hr=hr: e.tensor_scalar(hr, hr, mv2[0:64, 0:1], rs2[0:64, :], ALU.subtract, ALU.mult), r=[("hrow", b2), "mv2", "rs2"], w=[("hrow", b2)])
        P.op(POOL, lambda e, hr=hr: e.tensor_tensor(hr, hr, mlg_bc[0:64, :], ALU.mult), r=[("hrow", b2), "mlg_bc"], w=[("hrow", b2)])
        ym = yml[b2][0:64, :]
        P.op(POOL, lambda e, hr=hr, ym=ym, oj=oj: e.tensor_tensor(ym, hr, oj, ALU.mult), r=[("hrow", b2), ("oseg", sl)], w=[("yml", b2)])
        ytp = psb(1)[:, 512:768]
        for a in range(4):
            P.op(PE, lambda e, a=a, ym=ym, ytp=ytp: e.transpose(ytp[:, a * 64:(a + 1) * 64], ym[:, a * 128:(a + 1) * 128], ident_bf[0:64, 0:64]), r=[("yml", b2), "ident_bf"], w=[("ps", 1)])
        yT = ymlT[sl].rearrange("p (a t) -> p a t", a=4)
        P.op(ACT, lambda e, yT=yT, l=l, ytp=ytp: e.copy(yT[:, :, l * CS:(l + 1) * CS], ytp.rearrange("p (a t) -> p a t", a=4)), r=[("ps", 1)], w=[("ymlT", sl)])
        if l == SEGC - 1:
            for pc in range(GS // YW):
                tok0 = seg * GS + pc * YW
                P.op(SP, lambda e, yT=yT, pc=pc, q=tok0 // T, tl=tok0 % T: e.dma_start(out=y3[512:1024, q, tl:tl + YW].rearrange("(a p) t -> p a t", p=128), in_=yT[:, :, pc * YW:(pc + 1) * YW]),
                     r=[("ymlT", sl)], w=["y_d"], dma=True)
    P.barrier()
    A.release()

    if LV < 3:
        P.finish(list(dbg.keys()))
        return nc, P.emit(), list(dbg.keys()), in_names
    YCR = max(1, CCB // (T * 2))
    for k in range(4096 // YCR):
        P.op(POOL, lambda e, k=k: e.collective_compute("AllGather", ALU.bypass, replica_groups=GRP4, ins=[y_d[k * YCR:(k + 1) * YCR, :].opt()],
                                                       outs=[yall_d[k * RANKS * YCR:(k + 1) * RANKS * YCR, :].opt()]), r=["y_d"], w=["yall_d"], cc=True)
    if debug:
        P.op(SP, lambda e: e.dma_start(out=yall_dbg, in_=yall_d), r=["yall_d"], w=["yall_dbg"], dma=True)
    P.barrier()

    if LV < 4:
        P.finish(list(dbg.keys()))
        return nc, P.emit(), list(dbg.keys()), in_names
    ln_phase(x_own, NTT, TGS, hTt_d, h_d)

    rt_f = A.alloc(NTT * 4)
    rt_i = A.alloc(NTT * 2, I32)
    rtv = rt_f.rearrange("p (i k) -> p i k", k=4)
    riv = rt_i.rearrange("p (i k) -> p i k", k=2)
    ytab_t = A.alloc(32 * NTG, I32)
    P.op(SP, lambda e: e.dma_start(out=ytab_t, in_=ytab), w=["ytab"], dma=True)

    NPIECE = 4
    PIECE = 8 * 512

    A.mark()
    pieces = [A.alloc(PIECE, BF16) for _ in range(NPIECE)]
    pcount = [0]

    def load_piece(src_ap, skey):
        k = pcount[0] % NPIECE
        pcount[0] += 1
        pv = pieces[k].rearrange("p (c n) -> p c n", c=8)
        P.op(POOL, lambda e: e.dma_start(out=pv, in_=src_ap.rearrange("(c p) n -> p c n", p=128)), r=[skey], w=[("piece", k)], dma=True)
        return pv, ("piece", k)

    hTg2 = A.alloc(KD * TGS, BF16)
    hv2 = hTg2.rearrange("p (c t) -> p c t", c=KD)
    ybuf = A.alloc(32 * TGS, BF16)
    ybv = ybuf.rearrange("p (c t) -> p c t", c=32)
    sgt = A.alloc(4 * TGS)
    sgv = sgt.rearrange("p (m t) -> p m t", m=4)
    macc = A.alloc(4 * TGS)
    maccv = macc.rearrange("p (m t) -> p m t", m=4)
    mst = [A.alloc(4 * TGS, BF16) for _ in range(2)]
    NROWS_Y = RANKS * 4096
    bankset = [0]
    for tg in range(NTG):
        P.op(SP, lambda e, tg=tg: e.dma_start(out=hv2, in_=hTt_d[:, tg * TGS:(tg + 1) * TGS].rearrange("(c p) t -> p c t", p=128)), r=["hT_dst"], w=["hTg2"], dma=True)
        for k in range(32):
            if k < 16:
                col = (k // 4) * 8 + (k % 4)
            else:
                col = ((k - 16) // 4) * 8 + 4 + (k - 16) % 4
            P.op(POOL, lambda e, k=k, col=col, tg=tg: e.indirect_dma_start(out=ybv[:, k, :], out_offset=None, in_=yall2,
                                                                         in_offset=bass.IndirectOffsetOnAxis(ap=ytab_t[:, tg * 32 + col:tg * 32 + col + 1], axis=0)),
                 r=["yall_d", "ytab"], w=["ybuf"], dma=True)
        for mg in range(8):
            ms = mst[mg % 2].rearrange("p (m t) -> p m t", m=4)
            for qi, (wsrc, skey, coff, nk, rkey, rbuf, cbase) in enumerate((
                    (wgate, "wgate_f", mg * 512, 32, "hTg2", hv2, 0),
                    (wpf, "wpf_f", mg * 512, 16, "ybuf", ybv, 0),
                    (wgate, "wgate_f", D + mg * 512, 32, "hTg2", hv2, 0),
                    (wpm, "wpm_f", mg * 512, 16, "ybuf", ybv, 16))):
                bs = (bankset[0] % 2) * 4
                bankset[0] += 1
                for cb in range(nk // 8):
                    pv, pk = load_piece(wsrc[cb * 1024:(cb + 1) * 1024, coff:coff + 512], skey)
                    for m in range(4):
                        for cc in range(8):
                            c = cb * 8 + cc
                            P.op(PE, lambda e, m=m, cc=cc, c=c, pv=pv, bs=bs, rbuf=rbuf, cbase=cbase, nk=nk: e.matmul(
                                ps[bs + m][:, 0:TGS], pv[:, cc, m * 128:(m + 1) * 128], rbuf[:, cbase + c, :], start=(c == 0), stop=(c == nk - 1)),
                                r=[pk, rkey], w=[("ps", bs + m)])
                for m in range(4):
                    if qi in (0, 2):
                        P.op(ACT, lambda e, m=m, bs=bs: e.activation(sgv[:, m, :], ps[bs + m][:, 0:TGS], AF.Sigmoid), r=[("ps", bs + m)], w=[("sg", m)])
                    elif qi == 1:
                        P.op(DVE, lambda e, m=m, bs=bs: e.tensor_tensor(maccv[:, m, :], sgv[:, m, :], ps[bs + m][:, 0:TGS], ALU.mult), r=[("ps", bs + m), ("sg", m)], w=[("macc", m)])
                    else:
                        P.op(DVE, lambda e, m=m, bs=bs: e.tensor_tensor(sgv[:, m, :], sgv[:, m, :], ps[bs + m][:, 0:TGS], ALU.mult), r=[("ps", bs + m), ("sg", m)], w=[("sg", m)])
                        P.op(POOL, lambda e, m=m, ms=ms: e.tensor_tensor(ms[:, m, :], sgv[:, m, :], maccv[:, m, :], ALU.add), r=[("sg", m), ("macc", m)], w=[("mst", mg % 2)])
            P.op(SP, lambda e, ms=ms, mg=mg, tg=tg: e.dma_start(out=mT_d[mg * 512:(mg + 1) * 512, tg * TGS:(tg + 1) * TGS].rearrange("(m p) t -> p m t", p=128), in_=ms),
                 r=[("mst", mg % 2)], w=["mT_d"], dma=True)
    P.barrier()
    A.release()

    A.mark()
    pieces = [A.alloc(PIECE, BF16) for _ in range(NPIECE)]
    TG3 = min(256, T)
    TP3 = TG3 // 128
    mgT = A.alloc(KD * TG3, BF16)
    mgv = mgT.rearrange("p (c t) -> p c t", c=KD)
    g2_bc = A.alloc(D)
    b2_bc = A.alloc(D)
    P.op(SP, lambda e: e.dma_start(out=g2_bc, in_=lnp[2:3, :].to_broadcast([128, D])), w=["lnbc"], dma=True)
    P.op(SP, lambda e: e.dma_start(out=b2_bc, in_=lnp[3:4, :].to_broadcast([128, D])), w=["lnbc"], dma=True)
    r1 = [A.alloc(D) for _ in range(TP3)]
    hres = [A.alloc(512) for _ in range(4)]
    stats = A.alloc(48)
    mvt = A.alloc(2)
    rstd = A.alloc(1)
    hcnt = 0
    for tg in range(T // TG3):
        P.op(SP, lambda e, tg=tg: e.dma_start(out=mgv, in_=mT_d[:, tg * TG3:(tg + 1) * TG3].rearrange("(c p) t -> p c t", p=128)), r=["mT_d"], w=["mgT"], dma=True)
        for n in range(8):
            bs = (n % 2) * TP3
            for cb in range(4):
                pv, pk = load_piece(wo[cb * 1024:(cb + 1) * 1024, n * 512:(n + 1) * 512], "wo_f")
                for t in range(TP3):
                    for cc in range(8):
                        c = cb * 8 + cc
                        P.op(PE, lambda e, t=t, cc=cc, c=c, pv=pv, bs=bs: e.matmul(ps[bs + t], mgv[:, c, t * 128:(t + 1) * 128], pv[:, cc, :], start=(c == 0), stop=(c == KD - 1)),
                             r=[pk, "mgT"], w=[("ps", bs + t)])
            for t in range(TP3):
                hk = hcnt % 4
                hcnt += 1
                row0 = tg * TG3 + t * 128
                P.op(SP, lambda e, hk=hk, row0=row0, n=n: e.dma_start(out=hres[hk], in_=h_d[row0:row0 + 128, n * 512:(n + 1) * 512]), r=["h_dst"], w=[("hres", hk)], dma=True)
                P.op(DVE, lambda e, t=t, n=n, hk=hk, bs=bs: e.scalar_tensor_tensor(r1[t][:, n * 512:(n + 1) * 512], hres[hk], ALPHA, ps[bs + t], ALU.mult, ALU.add),
                     r=[("hres", hk), ("ps", bs + t)], w=[("r1", t)])
        for t in range(TP3):
            row0 = tg * TG3 + t * 128
            layer_norm_tile(r1[t], 0, g2_bc, b2_bc, [(r1[t], ("r1o", t))], ("r1", t), stats, mvt, rstd)
            P.op(SP, lambda e, t=t, row0=row0: e.dma_start(out=h1_d[row0:row0 + 128, :], in_=r1[t]), r=[("r1o", t), ("r1", t)], w=["h1_d", ("r1", t)], dma=True)
    P.barrier()
    A.release()

    if LV < 5:
        P.finish(list(dbg.keys()))
        return nc, P.emit(), list(dbg.keys()), in_names
    A.mark()
    h1t = [A.alloc(D) for _ in range(2)]
    h1b = [A.alloc(D, BF16) for _ in range(2)]
    h1T = A.alloc(KD * 128)
    h1Tv = h1T.rearrange("p (c t) -> p c t", c=KD)
    wr_t = A.alloc(KD * 72)
    wrv = wr_t.rearrange("p (c n) -> p c n", c=KD)
    br_bc = A.alloc(72)
    P.op(SP, lambda e: e.dma_start(out=wrv, in_=wr.rearrange("(c p) n -> p c n", p=128)), w=["wr"], dma=True)
    P.op(SP, lambda e: e.dma_start(out=br_bc, in_=br.to_broadcast([128, 72])), w=["br"], dma=True)
    iota64 = A.alloc(64)
    P.op(POOL, lambda e: e.iota(iota64, [[1, 64]], base=0, channel_multiplier=0, allow_small_or_imprecise_dtypes=True), w=["iota64"])
    lg = A.alloc(72)
    gm = A.alloc(8)
    pen = A.alloc(8)
    em = A.alloc(64)
    em2 = A.alloc(64)
    oh1 = A.alloc(64)
    oh2 = A.alloc(64)
    mb = A.alloc(64, BF16)
    cnt = A.alloc(64)
    rank = A.alloc(64)
    tmp64 = A.alloc(64)
    sc = A.alloc(16)
    P.op(DVE, lambda e: e.memset(cnt, 0.0), w=["cnt"])
    BIG = 1.0e30
    for i in range(NTT):
        b2 = i % 2
        ht, hb_ = h1t[b2], h1b[b2]
        P.op(SP, lambda e, ht=ht, i=i: e.dma_start(out=ht, in_=h1_d[i * 128:(i + 1) * 128, :]), r=["h1_d"], w=[("h1t", b2)], dma=True)
        P.op(ACT, lambda e, ht=ht, hb_=hb_: e.copy(hb_, ht), r=[("h1t", b2)], w=[("h1b", b2)])
        for q in range(8):
            bank = q % 4
            for cc in range(4):
                c = q * 4 + cc
                P.op(PE, lambda e, c=c, cc=cc, bank=bank, ht=ht: e.transpose(ps[bank][:, cc * 128:(cc + 1) * 128], ht[:, c * 128:(c + 1) * 128], ident_f), r=[("h1t", b2), "ident_f"], w=[("ps", bank)])
            P.op(ACT if q % 2 == 0 else DVE, (lambda e, q=q, bank=bank: e.copy(h1Tv[:, q * 4:(q + 1) * 4, :], ps[bank].rearrange("p (c t) -> p c t", c=4))) if q % 2 == 0 else
                 (lambda e, q=q, bank=bank: e.tensor_copy(h1Tv[:, q * 4:(q + 1) * 4, :], ps[bank].rearrange("p (c t) -> p c t", c=4))),
                 r=[("ps", bank)], w=["h1T"])
        for c in range(KD):
            P.op(PE, lambda e, c=c: e.matmul(ps[4][:, 0:72], h1Tv[:, c, :], wrv[:, c, :], start=(c == 0), stop=(c == KD - 1)), r=["h1T", "wr"], w=[("ps", 4)])
        P.op(DVE, lambda e: e.tensor_tensor(lg, ps[4][:, 0:72], br_bc, ALU.add), r=[("ps", 4), "br"], w=["lg"])
        P.op(DVE, lambda e: e.tensor_reduce(sc[:, 0:1], lg[:, 0:8], AX.X, ALU.max), r=["lg"], w=[("sc", 0)])
        P.op(DVE, lambda e: e.tensor_scalar(gm, lg[:, 0:8], sc[:, 0:1], None, ALU.is_equal), r=["lg", ("sc", 0)], w=["gm"])
        P.op(DVE, lambda e: e.tensor_scalar(sc[:, 1:2], sc[:, 0:1], -1.0, None, ALU.mult), r=[("sc", 0)], w=[("sc", 1)])
        P.op(ACT, lambda e: e.activation(pen, lg[:, 0:8], AF.Exp, bias=sc[:, 1:2], scale=1.0, accum_out=sc[:, 2:3]), r=["lg", ("sc", 1)], w=["pen", ("sc", 2)])
        P.op(DVE, lambda e: e.reciprocal(sc[:, 3:4], sc[:, 2:3]), r=[("sc", 2)], w=[("sc", 3)])
        P.op(DVE, lambda e: e.tensor_scalar(pen, gm, 1.0, BIG, ALU.subtract, ALU.mult), r=["gm", "pen"], w=["pen"])
        P.op(DVE, lambda e: e.tensor_tensor(em.rearrange("p (g k) -> p g k", k=8), lg[:, 8:72].rearrange("p (g k) -> p g k", k=8),
                                            pen.unsqueeze(2).to_broadcast([128, 8, 8]), ALU.add), r=["lg", "pen"], w=["em"])
        P.op(DVE, lambda e: e.tensor_reduce(sc[:, 4:5], em, AX.X, ALU.max), r=["em"], w=[("sc", 4)])
        P.op(DVE, lambda e: e.tensor_scalar(oh1, em, sc[:, 4:5], None, ALU.is_equal), r=["em", ("sc", 4)], w=["oh1"])
        P.op(DVE, lambda e: e.scalar_tensor_tensor(em2, oh1, -BIG, em, ALU.mult, ALU.add), r=["oh1", "em"], w=["em2"])
        P.op(DVE, lambda e: e.tensor_reduce(sc[:, 5:6], em2, AX.X, ALU.max), r=["em2"], w=[("sc", 5)])
        P.op(DVE, lambda e: e.tensor_scalar(oh2, em2, sc[:, 5:6], None, ALU.is_equal), r=["em2", ("sc", 5)], w=["oh2"])
        P.op(DVE, lambda e: e.tensor_tensor(sc[:, 6:7], sc[:, 5:6], sc[:, 4:5], ALU.subtract), r=[("sc", 4), ("sc", 5)], w=[("sc", 6)])
        P.op(ACT, lambda e: e.activation(sc[:, 6:7], sc[:, 6:7], AF.Exp), r=[("sc", 6)], w=[("sc", 6)])
        P.op(DVE, lambda e: e.tensor_scalar_add(sc[:, 7:8], sc[:, 6:7], 1.0), r=[("sc", 6)], w=[("sc", 7)])
        P.op(DVE, lambda e: e.reciprocal(sc[:, 7:8], sc[:, 7:8]), r=[("sc", 7)], w=[("sc", 7)])
        P.op(DVE, lambda e, i=i: e.tensor_tensor(rtv[:, i, 0:1], sc[:, 7:8], sc[:, 3:4], ALU.mult), r=[("sc", 7), ("sc", 3)], w=["rt_f"])
        P.op(DVE, lambda e, i=i: e.tensor_tensor(rtv[:, i, 1:2], rtv[:, i, 0:1], sc[:, 6:7], ALU.mult), r=["rt_f", ("sc", 6)], w=["rt_f"])
        P.op(DVE, lambda e: e.tensor_tensor(mb, oh1, oh2, ALU.add), r=["oh1", "oh2"], w=["mb"])
        P.op(PE, lambda e: e.matmul(ps[5][:, 0:64], tri_s, mb, start=True, stop=True), r=["tri_s", "mb"], w=[("ps", 5)])
        P.op(PE, lambda e: e.matmul(ps[6][:, 0:64], ones_bf, mb, start=True, stop=True), r=["ones_bf", "mb"], w=[("ps", 6)])
        P.op(DVE, lambda e: e.tensor_tensor(rank, ps[5][:, 0:64], cnt, ALU.add), r=[("ps", 5), "cnt"], w=["rank"])
        P.op(DVE, lambda e: e.tensor_tensor(cnt, ps[6][:, 0:64], cnt, ALU.add), r=[("ps", 6), "cnt", "rank"], w=["cnt"])
        P.op(DVE, lambda e: e.scalar_tensor_tensor(rank, iota64, float(CAP), rank, ALU.mult, ALU.add), r=["iota64", "rank"], w=["rank"])
        for k, oh in ((0, oh1), (1, oh2)):
            P.op(DVE, lambda e, oh=oh: e.tensor_tensor(tmp64, oh, rank, ALU.mult), r=["oh1", "oh2", "rank"], w=["tmp64"])
            P.op(DVE, lambda e, k=k: e.tensor_reduce(sc[:, 8 + k:9 + k], tmp64, AX.X, ALU.add), r=["tmp64"], w=[("sc", 8 + k)])
            P.op(DVE, lambda e, k=k, i=i: e.tensor_copy(riv[:, i, k:k + 1], sc[:, 8 + k:9 + k]), r=[("sc", 8 + k)], w=["rt_i"])
            P.op(POOL, lambda e, k=k, i=i, hb_=hb_: e.indirect_dma_start(out=xg_d, out_offset=bass.IndirectOffsetOnAxis(ap=riv[:, i, k:k + 1], axis=0), in_=hb_, in_offset=None),
                 r=["rt_i", ("h1b", b2)] + [("xg_d", z) for z in range(NE * CAP // 128)], w=["xg_sc"], dma=True)
        if debug:
            P.op(DVE, lambda e, i=i: e.tensor_copy(rtv[:, i, 2:3], sc[:, 8:9]), r=[("sc", 8)], w=["rt_f"])
            P.op(DVE, lambda e, i=i: e.tensor_copy(rtv[:, i, 3:4], sc[:, 9:10]), r=[("sc", 9)], w=["rt_f"])
            P.op(SP, lambda e, i=i: e.dma_start(out=rout_dbg[i * 128:(i + 1) * 128, :], in_=rtv[:, i, :]), r=["rt_f"], w=["rout_dbg"], dma=True)
    P.barrier()
    A.release()

    if LV < 6:
        P.finish(list(dbg.keys()))
        return nc, P.emit(), list(dbg.keys()), in_names
    A.mark()
    NPIECE = 6
    pieces = [A.alloc(PIECE, BF16) for _ in range(NPIECE)]
    xgt = [A.alloc(D, BF16) for _ in range(2)]
    xgT = [A.alloc(KD * 128, BF16) for _ in range(2)]
    sgl = A.alloc(512)
    aact = A.alloc(512, BF16)
    aT = A.alloc(4 * 128, BF16)
    aTv = aT.rearrange("p (f t) -> p f t", f=4)
    ogt = [A.alloc(D, BF16) for _ in range(2)]
    for ex in range(NE):
        b2 = ex % 2
        P.op(SP, lambda e, ex=ex, b2=b2: e.dma_start(out=xgt[b2], in_=xg_d[ex * CAP:(ex + 1) * CAP, :]), r=["xg_sc"], w=[("xgt", b2)], dma=True)
        xv = xgT[b2].rearrange("p (c t) -> p c t", c=KD)
        for q in range(4):
            for cc in range(8):
                c = q * 8 + cc
                P.op(PE, lambda e, q=q, cc=cc, c=c, b2=b2: e.transpose(psb(q)[:, cc * 128:(cc + 1) * 128], xgt[b2][:, c * 128:(c + 1) * 128], ident_bf), r=[("xgt", b2), "ident_bf"], w=[("ps", q)])
            P.op(ACT if q % 2 == 0 else DVE, (lambda e, q=q, xv=xv: e.copy(xv[:, q * 8:(q + 1) * 8, :], psb(q).rearrange("p (c t) -> p c t", c=8))) if q % 2 == 0 else
                 (lambda e, q=q, xv=xv: e.tensor_copy(xv[:, q * 8:(q + 1) * 8, :], psb(q).rearrange("p (c t) -> p c t", c=8))),
                 r=[("ps", q)], w=[("xgT", b2)])
        exl = ex % EPP
        for qi, (wsrc, skey) in enumerate(((wegp[ex // EPP], "weg%d_f" % (ex // EPP)), (weup[ex // EPP], "weu%d_f" % (ex // EPP)))):
            bank = 4 + qi
            for cb in range(4):
                pv, pk = load_piece(wsrc[exl * D + cb * 1024:exl * D + (cb + 1) * 1024, :], skey)
                for cc in range(8):
                    c = cb * 8 + cc
                    P.op(PE, lambda e, c=c, cc=cc, pv=pv, xv=xv, bank=bank: e.matmul(ps[bank], xv[:, c, :], pv[:, cc, :], start=(c == 0), stop=(c == KD - 1)), r=[pk, ("xgT", b2)], w=[("ps", bank)])
        P.op(ACT, lambda e: e.activation(sgl, ps[4], AF.Silu), r=[("ps", 4)], w=["sgl"])
        P.op(DVE, lambda e: e.tensor_tensor(aact, sgl, ps[5], ALU.mult), r=["sgl", ("ps", 5)], w=["aact"])
        for f in range(4):
            P.op(PE, lambda e, f=f: e.transpose(psb(6)[:, f * 128:(f + 1) * 128], aact[:, f * 128:(f + 1) * 128], ident_bf), r=["aact", "ident_bf"], w=[("ps", 6)])
        P.op(ACT, lambda e: e.copy(aTv, psb(6)[:, 0:512].rearrange("p (f t) -> p f t", f=4)), r=[("ps", 6)], w=["aT"])
        for k in range(4):
            kk_ = pcount[0] % NPIECE
            pcount[0] += 1
            pv = pieces[kk_].rearrange("p (f n) -> p f n", f=4)
            pk = ("piece", kk_)
            P.op(POOL, lambda e, pv=pv, ex=ex, k=k, exl=exl: e.dma_start(out=pv, in_=wedp[ex // EPP][exl * DE:(exl + 1) * DE, k * 1024:(k + 1) * 1024].rearrange("(f p) n -> p f n", p=128)), r=["wed%d_f" % (ex // EPP)], w=[pk], dma=True)
            for half in range(2):
                bank = 7 if (2 * k + half) % 2 == 0 else 3
                for f in range(4):
                    P.op(PE, lambda e, f=f, pv=pv, half=half, bank=bank: e.matmul(ps[bank], aTv[:, f, :], pv[:, f, half * 512:(half + 1) * 512], start=(f == 0), stop=(f == 3)), r=[pk, "aT"], w=[("ps", bank)])
                col0 = k * 1024 + half * 512
                if half == 0:
                    P.op(ACT, lambda e, bank=bank, col0=col0, b2=b2: e.copy(ogt[b2][:, col0:col0 + 512], ps[bank]), r=[("ps", bank)], w=[("ogt", b2)])
                else:
                    P.op(DVE, lambda e, bank=bank, col0=col0, b2=b2: e.tensor_copy(ogt[b2][:, col0:col0 + 512], ps[bank]), r=[("ps", bank)], w=[("ogt", b2)])
        P.op(SP, lambda e, ex=ex, b2=b2: e.dma_start(out=og_d[ex * CAP:(ex + 1) * CAP, :], in_=ogt[b2]), r=[("ogt", b2)], w=["og_d"], dma=True)
    P.barrier()
    A.release()

    A.mark()
    g3_bc = A.alloc(D)
    b3_bc = A.alloc(D)
    P.op(SP, lambda e: e.dma_start(out=g3_bc, in_=lnp[4:5, :].to_broadcast([128, D])), w=["lnbc"], dma=True)
    P.op(SP, lambda e: e.dma_start(out=b3_bc, in_=lnp[5:6, :].to_broadcast([128, D])), w=["lnbc"], dma=True)
    rowA = [A.alloc(D, BF16) for _ in range(2)]
    rowB = [A.alloc(D, BF16) for _ in range(2)]
    h1t = [A.alloc(D) for _ in range(2)]
    acc = [A.alloc(D) for _ in range(2)]
    stats = A.alloc(48)
    mvt = A.alloc(2)
    rstd = A.alloc(1)
    for i in range(NTT):
        b2 = i % 2
        P.op(POOL, lambda e, i=i, b2=b2: e.indirect_dma_start(out=rowA[b2], out_offset=None, in_=og_d, in_offset=bass.IndirectOffsetOnAxis(ap=riv[:, i, 0:1], axis=0)), r=["og_d", "rt_i"], w=[("rowA", b2)], dma=True)
        P.op(POOL, lambda e, i=i, b2=b2: e.indirect_dma_start(out=rowB[b2], out_offset=None, in_=og_d, in_offset=bass.IndirectOffsetOnAxis(ap=riv[:, i, 1:2], axis=0)), r=["og_d", "rt_i"], w=[("rowB", b2)], dma=True)
        P.op(SP, lambda e, i=i, b2=b2: e.dma_start(out=h1t[b2], in_=h1_d[i * 128:(i + 1) * 128, :]), r=["h1_d"], w=[("h1t", b2)], dma=True)
        P.op(DVE, lambda e, i=i, b2=b2: e.tensor_scalar(acc[b2], rowA[b2], rtv[:, i, 0:1], None, ALU.mult), r=[("rowA", b2), "rt_f"], w=[("acc", b2)])
        P.op(DVE, lambda e, i=i, b2=b2: e.scalar_tensor_tensor(acc[b2], rowB[b2], rtv[:, i, 1:2], acc[b2], ALU.mult, ALU.add), r=[("rowB", b2), "rt_f", ("acc", b2)], w=[("acc", b2)])
        if debug:
            P.op(SP, lambda e, i=i, b2=b2: e.dma_start(out=ffn_dbg[i * 128:(i + 1) * 128, :], in_=acc[b2]), r=[("acc", b2)], w=["ffn_dbg"], dma=True)
        P.op(DVE, lambda e, b2=b2: e.scalar_tensor_tensor(acc[b2], h1t[b2], ALPHA, acc[b2], ALU.mult, ALU.add), r=[("h1t", b2), ("acc", b2)], w=[("acc", b2)])
        layer_norm_tile(acc[b2], 0, g3_bc, b3_bc, [(acc[b2], ("acco", b2))], ("acc", b2), stats, mvt, rstd)
        P.op(SP, lambda e, i=i, b2=b2: e.dma_start(out=out[i * 128:(i + 1) * 128, :], in_=acc[b2]), r=[("acco", b2), ("acc", b2)], w=["out", ("acc", b2)], dma=True)
    P.finish(["out"] + list(dbg.keys()))
    counts = P.emit()
    return nc, counts, list(dbg.keys()), in_names


def make_in_maps(S, names, x, ln_in_g, ln_in_b, w_in, b_fox_f, b_ml_i, b_ml_f, conv_w, conv_b, ml_norm_g, w_proj_fox, w_proj_ml, w_out,
                 ln_mix_g, ln_mix_b, w_group, b_group, w_router, b_router, w_gate, w_up, w_down, ln_moe_g, ln_moe_b):
    f = lambda a: np.ascontiguousarray(np.asarray(a, dtype=np.float32))
    T = S // RANKS
    w_in = f(w_in)[0]
    lnp = f(np.stack([f(ln_in_g), f(ln_in_b), f(ln_mix_g)[0], f(ln_mix_b)[0], f(ln_moe_g)[0], f(ln_moe_b)[0]], 0))
    shared = {
        "wgate": w_in[:, O_GA:O_GA + 2 * D], "wpf": f(w_proj_fox)[0], "wpm": f(w_proj_ml)[0], "wo": f(w_out)[0],
    }
    wg_, wu_, wd_ = (np.asarray(a_, dtype=np.float32)[0] for a_ in (w_gate, w_up, w_down))
    for p_ in range(NE // 16):
        shared["weg%d" % p_] = wg_[p_ * 16:(p_ + 1) * 16].reshape(16 * D, DE)
        shared["weu%d" % p_] = wu_[p_ * 16:(p_ + 1) * 16].reshape(16 * D, DE)
        shared["wed%d" % p_] = wd_[p_ * 16:(p_ + 1) * 16].reshape(16 * DE, D)
    wr = f(np.concatenate([f(w_group)[0], f(w_router)[0]], 1))
    br = f(np.concatenate([f(b_group)[0], f(b_router)[0]], 0)[None, :])
    conv_w, conv_b = f(conv_w)[0], f(conv_b)[0]
    x = np.asarray(x, dtype=np.float32)
    xcr = max(1, (512 * 1024) // (D * 4))
    maps = []
    for c in range(NCORE):
        b, r = c // RANKS, c % RANKS
        hs = slice(r * 512, (r + 1) * 512)
        wa = np.concatenate([w_in[:, O_FQ + r * 512:O_FQ + (r + 1) * 512], w_in[:, O_FK + r * 512:O_FK + (r + 1) * 512],
                             w_in[:, O_MQ + r * 256:O_MQ + (r + 1) * 256], w_in[:, O_MK + r * 256:O_MK + (r + 1) * 256]], 1)
        wb = np.concatenate([w_in[:, O_FV + r * 512:O_FV + (r + 1) * 512], w_in[:, O_MV + r * 512:O_MV + (r + 1) * 512],
                             w_in[:, O_MO + r * 512:O_MO + (r + 1) * 512]], 1)
        wg6 = np.concatenate([w_in[:, O_FF + 4 * r:O_FF + 4 * r + 4], w_in[:, O_MF + r:O_MF + r + 1], w_in[:, O_MI + r:O_MI + r + 1]], 1)
        gb6 = np.concatenate([f(b_fox_f)[0, 4 * r:4 * r + 4], f(b_ml_f)[0, r:r + 1], f(b_ml_i)[0, r:r + 1]])[:, None]
        cw = np.concatenate([conv_w[:, r * 256:(r + 1) * 256], conv_w[:, 1024 + r * 256:1024 + (r + 1) * 256]], 1).T
        cb = np.concatenate([conv_b[r * 256:(r + 1) * 256], conv_b[1024 + r * 256:1024 + (r + 1) * 256]])
        ntg = T // min(512, T)
        ytab = np.zeros((128, 32 * ntg), np.int32)
        ycr = max(1, (512 * 1024) // (T * 2))
        for tg_ in range(ntg):
            for i in range(RANKS):
                for ct in range(8):
                    L = (ct * 128 + np.arange(128)) * 4 + r
                    ytab[:, tg_ * 32 + i * 8 + ct] = ((L // ycr) * (RANKS * ycr) + i * ycr + (L % ycr)) * ntg + tg_
        m = {
            "x": f(x[b]), "x_own": f(x[b, r * T:(r + 1) * T]), "lnp": lnp, "wa": f(wa), "wb": f(wb), "wg6": f(wg6), "gb6": f(gb6),
            "convw": f(cw.reshape(4, 128, 4).transpose(1, 0, 2).reshape(128, 16)), "convb": f(cb.reshape(4, 128).T),
            "mlg": f(f(ml_norm_g)[0, hs][None, :]), "wr": wr, "br": br, "ytab": ytab,
        }
        for k, v in shared.items():
            if k + "_s" in names:
                R_, C_ = v.shape
                rows = R_ // NCORE
                cr = max(1, (512 * 1024) // (C_ * 4))
                m[k + "_s"] = f(v.reshape(rows // cr, NCORE, cr, C_)[:, c].reshape(rows, C_))
        maps.append({k: v for k, v in m.items() if k in names})
    return maps


_CACHE = {}


def kernel(**inputs):
    x = np.asarray(inputs["x"])
    B, S, _ = x.shape
    T = S // RANKS
    if S not in _CACHE:
        r_ = build_program(S)
        _CACHE[S] = (r_[0], r_[3])
    nc, names = _CACHE[S]
    maps = make_in_maps(S, names, **inputs)
    res = run_bass_kernel_spmd(nc, maps, core_ids=list(range(NCORE)))
    out = np.zeros((B, S, D), np.float32)
    for c in range(NCORE):
        b, r = c // RANKS, c % RANKS
        out[b, r * T:(r + 1) * T] = np.asarray(res.results[c]["out"])
    return out
```
